# Optimizing a Trainium2 kernel written in Bass

```python
import math
import jax, jax.numpy as jnp
from jax import lax
import numpy as np

D_MODEL = 2048
BATCH = 2
SEQ = 8192
DEPTH = 4

HEAD_DIM = 64
N_MIXERS = 4
GROUP_HEADS = D_MODEL // (N_MIXERS * HEAD_DIM)
GROUP_WIDTH = GROUP_HEADS * HEAD_DIM
MIX_WIDTH = N_MIXERS * GROUP_WIDTH
IDX_HEADS = 16
IDX_DIM = 64
TOPK_MAX = 256
WINDOW = 128
BLOCK_Q = 128
REL_BUCKETS = 32
REL_MAX_DIST = 128
D_FF = 11 * D_MODEL // 4
EPS = 1e-6

SPLIT_SIZES = (
    GROUP_WIDTH, HEAD_DIM, HEAD_DIM, IDX_HEADS * IDX_DIM, IDX_DIM, IDX_HEADS,
    GROUP_WIDTH, HEAD_DIM, HEAD_DIM,
    GROUP_WIDTH, GROUP_WIDTH, GROUP_WIDTH, GROUP_HEADS, GROUP_WIDTH,
    GROUP_WIDTH, GROUP_WIDTH, GROUP_WIDTH,
)
IN_COLS = sum(SPLIT_SIZES)

kernel_name = 'hybrid_dsa_swa_fox_stickbreak_macaron'


def _rms(x, g):
    xf = x.astype(jnp.float32)
    y = xf * lax.rsqrt(jnp.mean(jnp.square(xf), axis=-1, keepdims=True) + EPS)
    return (y * g.astype(jnp.float32)).astype(x.dtype)


def _modulate(x, g, shift, scale):
    return _rms(x, g) * (1 + scale[:, None, :]) + shift[:, None, :]


def _swiglu(h, w_gate, w_up, w_down):
    return (jax.nn.silu(h @ w_gate) * (h @ w_up)) @ w_down


def _to_blocks(a):
    b, s = a.shape[:2]
    a = a.reshape((b, s // BLOCK_Q, BLOCK_Q) + a.shape[2:])
    return jnp.moveaxis(a, 1, 0)


def _from_blocks(a):
    a = jnp.moveaxis(a, 0, 1)
    return a.reshape((a.shape[0], a.shape[1] * a.shape[2]) + a.shape[3:])


def _rel_bucket(dist):
    n = jnp.maximum(dist, 0)
    max_exact = REL_BUCKETS // 2
    nf = jnp.maximum(n, 1).astype(jnp.float32)
    large = max_exact + (jnp.log(nf / max_exact) / math.log(REL_MAX_DIST / max_exact)
                         * (REL_BUCKETS - max_exact)).astype(jnp.int32)
    large = jnp.minimum(large, REL_BUCKETS - 1)
    return jnp.where(n < max_exact, n, large)


def _dsa_attention(q, k, v, iq, ik, iw, rel_tab):
    seq = k.shape[1]
    k_top = min(TOPK_MAX, seq // 4)
    kpos = jnp.arange(seq)
    scale = HEAD_DIM ** -0.5
    gather = jax.vmap(lambda t, i: t[i])

    def block(args):
        blk, q_b, iq_b, iw_b = args
        qpos = blk * BLOCK_Q + jnp.arange(BLOCK_Q)
        rel = jax.nn.relu(jnp.einsum('bqhd,bsd->bqhs', iq_b, ik).astype(jnp.float32) * IDX_DIM ** -0.5)
        score = jnp.einsum('bqhs,bqh->bqs', rel, iw_b.astype(jnp.float32))
        score = jnp.where(kpos[None, None, :] <= qpos[None, :, None], score, -jnp.inf)
        _, sel = lax.top_k(score, k_top)
        k_sel = gather(k, sel)
        v_sel = gather(v, sel)
        logits = jnp.einsum('bqhd,bqkd->bhqk', q_b, k_sel).astype(jnp.float32) * scale
        dist = qpos[None, :, None] - sel
        bias = jnp.moveaxis(rel_tab[_rel_bucket(dist)], -1, 1).astype(jnp.float32)
        logits = jnp.where((dist >= 0)[:, None], logits + bias, -jnp.inf)
        p = jax.nn.softmax(logits, axis=-1).astype(v.dtype)
        return jnp.einsum('bhqk,bqkd->bqhd', p, v_sel)

    nblk = seq // BLOCK_Q
    out = lax.map(block, (jnp.arange(nblk), _to_blocks(q), _to_blocks(iq), _to_blocks(iw)))
    return _from_blocks(out)


def _swa_sink_attention(q, k, v, sinks, rel_tab):
    b, seq, h, dh = q.shape
    n = seq // BLOCK_Q
    qb = q.reshape(b, n, BLOCK_Q, h, dh)

    def band(t):
        tb = t.reshape(b, n, BLOCK_Q, dh)
        prev = jnp.pad(tb, ((0, 0), (1, 0), (0, 0), (0, 0)))[:, :-1]
        return jnp.concatenate([prev, tb], axis=2)

    kk, vv = band(k), band(v)
    logits = jnp.einsum('bnqhd,bnkd->bnhqk', qb, kk).astype(jnp.float32) * HEAD_DIM ** -0.5
    dist = jnp.arange(BLOCK_Q)[:, None] + BLOCK_Q - jnp.arange(2 * BLOCK_Q)[None, :]
    kpos = jnp.arange(n)[:, None] * BLOCK_Q - BLOCK_Q + jnp.arange(2 * BLOCK_Q)[None, :]
    valid = ((dist >= 0) & (dist < WINDOW))[None] & (kpos >= 0)[:, None, :]
    bias = jnp.moveaxis(rel_tab[_rel_bucket(dist)], -1, 0).astype(jnp.float32)
    logits = jnp.where(valid[None, :, None], logits + bias, -jnp.inf)
    sink = jnp.broadcast_to(sinks.astype(jnp.float32)[None, None, :, None, None], logits.shape[:-1] + (1,))
    p = jax.nn.softmax(jnp.concatenate([logits, sink], axis=-1), axis=-1)[..., :-1]
    o = jnp.einsum('bnhqk,bnkd->bnqhd', p.astype(v.dtype), vv)
    return o.reshape(b, seq, h, dh)


def _forgetting_attention(q, k, v, log_f):
    seq = k.shape[1]
    cum = jnp.cumsum(log_f, axis=1)
    cum_k = jnp.moveaxis(cum, 1, 2)
    kpos = jnp.arange(seq)

    def block(args):
        blk, q_b, cum_q = args
        qpos = blk * BLOCK_Q + jnp.arange(BLOCK_Q)
        logits = jnp.einsum('bqhd,bshd->bhqs', q_b, k).astype(jnp.float32) * HEAD_DIM ** -0.5
        logits = logits + jnp.moveaxis(cum_q, 1, 2)[..., None] - cum_k[:, :, None, :]
        logits = jnp.where(kpos[None, :] <= qpos[:, None], logits, -jnp.inf)
        p = jax.nn.softmax(logits, axis=-1).astype(v.dtype)
        return jnp.einsum('bhqs,bshd->bqhd', p, v)

    nblk = seq // BLOCK_Q
    out = lax.map(block, (jnp.arange(nblk), _to_blocks(q), _to_blocks(cum)))
    return _from_blocks(out)


def _stick_breaking_attention(q, k, v):
    seq = k.shape[1]
    kpos = jnp.arange(seq)

    def block(args):
        blk, q_b = args
        qpos = blk * BLOCK_Q + jnp.arange(BLOCK_Q)
        z = jnp.einsum('bqhd,bshd->bhqs', q_b, k).astype(jnp.float32) * HEAD_DIM ** -0.5
        before = kpos[None, :] < qpos[:, None]
        u = jnp.where(before, jax.nn.log_sigmoid(-z), 0.0)
        between = lax.cumsum(u, axis=3, reverse=True) - u
        w = jnp.where(before, jnp.exp(jax.nn.log_sigmoid(z) + between), 0.0)
        return jnp.einsum('bhqs,bshd->bqhd', w.astype(v.dtype), v)

    nblk = seq // BLOCK_Q
    out = lax.map(block, (jnp.arange(nblk), _to_blocks(q)))
    return _from_blocks(out)


def _mixer(h, w_in, qk_g, forget_b, sinks, rel_table, group_g, w_out):
    b, seq, _ = h.shape
    points = [int(p) for p in np.cumsum(SPLIT_SIZES)[:-1]]
    (a_q, a_k, a_v, a_iq, a_ik, a_iw,
     b_q, b_k, b_v,
     c_q, c_k, c_v, c_f, c_g,
     d_q, d_k, d_v) = jnp.split(h @ w_in, points, axis=-1)
    heads = lambda t: t.reshape(b, seq, GROUP_HEADS, HEAD_DIM)
    o_a = _dsa_attention(_rms(heads(a_q), qk_g[0]), _rms(a_k, qk_g[1]), a_v,
                         a_iq.reshape(b, seq, IDX_HEADS, IDX_DIM), a_ik, a_iw * IDX_HEADS ** -0.5,
                         rel_table[:, :GROUP_HEADS])
    o_b = _swa_sink_attention(_rms(heads(b_q), qk_g[2]), _rms(b_k, qk_g[3]), b_v, sinks,
                              rel_table[:, GROUP_HEADS:])
    log_f = jax.nn.log_sigmoid(c_f.astype(jnp.float32) + forget_b.astype(jnp.float32))
    o_c = _forgetting_attention(_rms(heads(c_q), qk_g[4]), _rms(heads(c_k), qk_g[5]), heads(c_v), log_f)
    o_c = o_c * jax.nn.sigmoid(heads(c_g))
    o_d = _stick_breaking_attention(heads(d_q), heads(d_k), heads(d_v))
    y = jnp.stack([o.reshape(b, seq, GROUP_WIDTH) for o in (o_a, o_b, o_c, o_d)], axis=2)
    y = _rms(y, group_g.reshape(N_MIXERS, GROUP_WIDTH)).reshape(b, seq, MIX_WIDTH)
    return y @ w_out


def setup_inputs(seed: int = 0) -> dict:
    key = jax.random.key(seed)
    ks = jax.random.split(key, 16)
    nrm = lambda k, shape, s: jax.random.normal(k, shape, jnp.float32) * s
    return {
        'x': nrm(ks[0], (BATCH, SEQ, D_MODEL), 1.0),
        'c': nrm(ks[1], (BATCH, D_MODEL), 1.0),
        'w_ada': nrm(ks[2], (DEPTH, D_MODEL, 9 * D_MODEL), 0.5 * D_MODEL ** -0.5),
        'b_ada': nrm(ks[3], (DEPTH, 9 * D_MODEL), 0.02),
        'norm_g': 1.0 + nrm(ks[4], (DEPTH, 3, D_MODEL), 0.02),
        'w_in': nrm(ks[5], (DEPTH, D_MODEL, IN_COLS), D_MODEL ** -0.5),
        'qk_g': 1.0 + nrm(ks[6], (DEPTH, 6, HEAD_DIM), 0.02),
        'forget_b': 4.0 + nrm(ks[7], (DEPTH, GROUP_HEADS), 0.5),
        'sinks': nrm(ks[8], (DEPTH, GROUP_HEADS), 0.5),
        'rel_table': nrm(ks[9], (REL_BUCKETS, 2 * GROUP_HEADS), 0.5),
        'group_g': 1.0 + nrm(ks[10], (DEPTH, MIX_WIDTH), 0.02),
        'w_out': nrm(ks[11], (DEPTH, MIX_WIDTH, D_MODEL), MIX_WIDTH ** -0.5),
        'w_ffn_gate': nrm(ks[12], (DEPTH, 2, D_MODEL, D_FF), D_MODEL ** -0.5),
        'w_ffn_up': nrm(ks[13], (DEPTH, 2, D_MODEL, D_FF), D_MODEL ** -0.5),
        'w_ffn_down': nrm(ks[14], (DEPTH, 2, D_FF, D_MODEL), D_FF ** -0.5),
    }


def reference(x, c, w_ada, b_ada, norm_g, w_in, qk_g, forget_b, sinks, rel_table, group_g, w_out,
              w_ffn_gate, w_ffn_up, w_ffn_down):
    cond = jax.nn.silu(c)
    for l in range(DEPTH):
        mod = cond @ w_ada[l] + b_ada[l]
        sh1, sc1, g1, sh2, sc2, g2, sh3, sc3, g3 = jnp.split(mod, 9, axis=-1)
        h = _modulate(x, norm_g[l, 0], sh1, sc1)
        x = x + 0.5 * g1[:, None] * _swiglu(h, w_ffn_gate[l, 0], w_ffn_up[l, 0], w_ffn_down[l, 0])
        h = _modulate(x, norm_g[l, 1], sh2, sc2)
        x = x + g2[:, None] * _mixer(h, w_in[l], qk_g[l], forget_b[l], sinks[l], rel_table,
                                     group_g[l], w_out[l])
        h = _modulate(x, norm_g[l, 2], sh3, sc3)
        x = x + 0.5 * g3[:, None] * _swiglu(h, w_ffn_gate[l, 1], w_ffn_up[l, 1], w_ffn_down[l, 1])
    return x
```

```python
import math
from contextlib import ExitStack

import numpy as np
import concourse.bass as bass
import concourse.mybir as mybir
from concourse.bass_utils import run_bass_kernel_spmd

F32 = mybir.dt.float32
BF16 = mybir.dt.bfloat16
AF = mybir.ActivationFunctionType
ALU = mybir.AluOpType

D = 2048
KC = 16
DFF = 5632
FC = 44
SEQ = 8192
NBLK = 64
NLOC = 16
TOK = 2048
INC = 5976
EPS = 1e-6
NEG = -30000.0
TOPK = 256
NBIS = 22

ENG_NAMES = ("tensor", "vector", "scalar", "gpsimd", "sync")
DMA_RING = 8
SEM_CHUNK = 30000


class Buf:
    __slots__ = ("name", "last_w", "readers")

    def __init__(self, name):
        self.name = name
        self.last_w = None
        self.readers = []


class Op:
    __slots__ = ("eng", "fn", "deps", "kind", "needs_inc", "sem_i", "sem_v", "inc_sem", "inc_val")

    def __init__(self, eng, fn, kind):
        self.eng = eng
        self.fn = fn
        self.deps = []
        self.kind = kind
        self.needs_inc = False
        self.sem_i = None
        self.sem_v = None
        self.inc_sem = None
        self.inc_val = None


class Prog:
    def __init__(self, nc):
        self.nc = nc
        self.ops = {e: [] for e in ENG_NAMES}
        self.dma_count = {e: 0 for e in ENG_NAMES}
        self.cc_count = 0
        self.pending_dma = []
        self.nops = 0

    def _add(self, eng, fn, reads, writes, kind):
        op = Op(eng, fn, kind)
        deps = []
        for b in reads:
            if b.last_w is not None:
                deps.append(b.last_w)
        for b in writes:
            if b.last_w is not None:
                deps.append(b.last_w)
            deps.extend(b.readers)
        seen = set()
        for d in deps:
            if id(d) in seen:
                continue
            seen.add(id(d))
            if d.kind == "c" and kind == "c" and d.eng == eng == "tensor":
                continue
            if d.kind == "w":
                continue
            op.deps.append(d)
            if d.kind == "c":
                d.needs_inc = True
        for b in reads:
            b.readers.append(op)
        for b in writes:
            b.last_w = op
            b.readers = []
        if kind == "d":
            i = self.dma_count[eng]
            self.dma_count[eng] += 1
            op.sem_i = i % DMA_RING
            op.sem_v = 16 * (i // DMA_RING + 1)
            self.pending_dma.append(op)
        elif kind == "cc":
            self.cc_count += 1
            op.sem_v = self.cc_count
            self.pending_dma.append(op)
        self.ops[eng].append(op)
        self.nops += 1
        return op

    def op(self, eng, fn, reads=(), writes=()):
        return self._add(eng, fn, reads, writes, "c")

    def dma(self, eng, fn, reads=(), writes=()):
        return self._add(eng, fn, reads, writes, "d")

    def cc(self, fn, reads=(), writes=()):
        return self._add("gpsimd", fn, reads, writes, "cc")

    def barrier(self):
        lasts = []
        for e in ENG_NAMES:
            for o in reversed(self.ops[e]):
                if o.kind == "c":
                    lasts.append(o)
                    o.needs_inc = True
                    break
        pend = list(self.pending_dma)
        self.pending_dma = []
        for e in ENG_NAMES:
            w = Op(e, None, "w")
            w.deps = [d for d in lasts if d.eng != e] + pend
            self.ops[e].append(w)

    def emit(self, final_waits=()):
        nc = self.nc
        with ExitStack() as st:
            for d in final_waits:
                if d.kind == "c":
                    d.needs_inc = True
            for e in ENG_NAMES:
                n_inc = sum(1 for o in self.ops[e] if o.needs_inc)
                nsem = max(1, (n_inc + SEM_CHUNK - 1) // SEM_CHUNK)
                sems = [st.enter_context(nc.semaphore(f"s_{e}_{k}")) for k in range(nsem)]
                c = 0
                for o in self.ops[e]:
                    if o.needs_inc:
                        o.inc_sem = sems[c // SEM_CHUNK]
                        o.inc_val = c % SEM_CHUNK + 1
                        c += 1
            dma_sems = {}
            for e in ENG_NAMES:
                if self.dma_count[e] > 0:
                    dma_sems[e] = [st.enter_context(nc.semaphore(f"d_{e}_{k}")) for k in range(DMA_RING)]
            cc_sem = st.enter_context(nc.semaphore("ccsem"))
            block = st.enter_context(nc.Block())

            def make(e):
                def body(eng):
                    waited = {}

                    def wait(sem, val):
                        k = id(sem)
                        if waited.get(k, 0) >= val:
                            return
                        waited[k] = val
                        eng.wait_ge(sem, val)

                    def wait_dep(d):
                        if d.kind == "d":
                            wait(dma_sems[d.eng][d.sem_i], d.sem_v)
                        elif d.kind == "cc":
                            wait(cc_sem, d.sem_v)
                        else:
                            wait(d.inc_sem, d.inc_val)

                    for o in self.ops[e]:
                        for d in o.deps:
                            wait_dep(d)
                        if o.kind == "w":
                            continue
                        if o.kind == "d" and o.sem_v > 16:
                            wait(dma_sems[e][o.sem_i], o.sem_v - 16)
                        ins = o.fn(eng)
                        if o.kind == "d":
                            ins.then_inc(dma_sems[e][o.sem_i], 16)
                        elif o.kind == "cc":
                            ins.then_inc(cc_sem)
                        elif o.needs_inc:
                            ins.then_inc(o.inc_sem, 1)
                    if e == "sync":
                        for d in final_waits:
                            wait_dep(d)
                return body

            for e in ENG_NAMES:
                getattr(block, e)(make(e))


class Arena:
    def __init__(self, ap, nwords):
        self.ap = ap
        self.n = nwords
        self.off = 0

    def mark(self):
        return self.off

    def release(self, m):
        self.off = m

    def alloc(self, free_shape, dtype, parts=128):
        nelem = int(np.prod(free_shape))
        esz = 4 if dtype == F32 else 2
        nw = (nelem * esz + 31) // 32 * 8
        assert self.off + nw <= self.n, f"arena overflow {self.off}+{nw}>{self.n}"
        v = self.ap[:, self.off:self.off + nw]
        self.off += nw
        if dtype != F32:
            v = v.bitcast(dtype)
        v = v[:, :nelem]
        if len(free_shape) == 2:
            v = v.rearrange("p (a b) -> p a b", b=free_shape[1])
        elif len(free_shape) == 3:
            v = v.rearrange("p (a b c) -> p a b c", b=free_shape[1], c=free_shape[2])
        if parts != 128:
            v = v[0:parts]
        return v


def loc2blk(r, i):
    return 8 * (i // 2) + (r if i % 2 == 0 else 7 - r)


def blk2loc(j):
    m = j % 8
    if m < 4:
        return m, 2 * (j // 8)
    return 7 - m, 2 * (j // 8) + 1


def rel_bucket(d):
    if d < 16:
        return d
    v = 16 + int(np.float32(np.log(np.float32(d) / np.float32(16.0))) / np.float32(math.log(8.0)) * np.float32(16))
    return min(v, 31)


C_AQ, C_AK, C_AV, C_AIQ, C_AIK, C_AIW = 0, 512, 576, 640, 1664, 1728
C_BQ, C_BK, C_BV = 1744, 2256, 2320
C_CQ, C_CK, C_CV, C_CF, C_CG = 2384, 2896, 3408, 3920, 3928
C_DQ, C_DK, C_DV = 4440, 4952, 5464
T_AK, T_AIK, T_BK, T_CK, T_DK, T_ROWS = 0, 64, 128, 192, 704, 1216
V_A, V_B, V_C, V_D, V_COLS = 0, 64, 128, 640, 1152

K_ID, K_ONES, K_TINC, K_CAUSN, K_CAUST, K_STRICT, K_STRNEG, K_BLK, K_OH, K_INV = (
    0, 128, 256, 384, 512, 640, 768, 896, 1024, 1024 + 8192)
NCONST = K_INV + 256


def make_consts():
    c = np.zeros((128, NCONST), np.float32)
    i = np.arange(128)
    c[:, K_ID:K_ID + 128] = np.eye(128)
    c[:, K_ONES:K_ONES + 128] = 1.0
    c[:, K_TINC:K_TINC + 128] = -(i[:, None] >= i[None, :]).astype(np.float32)
    c[:, K_CAUSN:K_CAUSN + 128] = np.where(i[None, :] > i[:, None], -1e30, 0.0)
    c[:, K_CAUST:K_CAUST + 128] = np.where(i[:, None] > i[None, :], NEG, 0.0)
    c[:, K_STRICT:K_STRICT + 128] = (i[:, None] < i[None, :]).astype(np.float32)
    c[:, K_STRNEG:K_STRNEG + 128] = np.where(i[:, None] < i[None, :], 0.0, NEG)
    c[:, K_BLK:K_BLK + 128] = ((i[:, None] // 64) == (i[None, :] // 64)).astype(np.float32) / 64.0
    for typ in range(2):
        for s in range(128):
            for q in range(128):
                d = q - s + (128 if typ == 1 else 0)
                if 0 <= d < 128:
                    c[s, K_OH + (typ * 32 + rel_bucket(d)) * 128 + q] = 1.0
                else:
                    c[s, K_INV + typ * 128 + q] = 1.0
    return c


def build_program(depth, stop=99):
    nc = bass.Bass("TRN2", target_bir_lowering=False)
    P = Prog(nc)

    def dram(name, shape, dt, kind="Internal"):
        return nc.dram_tensor(name, list(shape), dt, kind=kind).ap()

    x_in = dram("x", [TOK, D], F32, "ExternalInput")
    cT_in = dram("cT", [128, KC], F32, "ExternalInput")
    bada_in = dram("badaT", [depth, 128, 144], F32, "ExternalInput")
    ng_in = dram("normgT", [depth, 3, 128, KC], F32, "ExternalInput")
    qkg_in = dram("qkg", [depth, 128, 6], F32, "ExternalInput")
    fb_in = dram("fb", [depth, 8, 1], F32, "ExternalInput")
    sinks_in = dram("sinks", [depth, 8], F32, "ExternalInput")
    tab_in = dram("tab", [32, 16], F32, "ExternalInput")
    gg_in = dram("gg", [depth, D], F32, "ExternalInput")
    consts_in = dram("consts", [128, NCONST], F32, "ExternalInput")
    wshard = {}
    wfull = {}
    wspec = [("wada0", 256, 9216), ("wada1", 256, 9216), ("win", 256, INC), ("wout", 256, D),
             ("wg0", 256, DFF), ("wu0", 256, DFF), ("wd0", 704, D),
             ("wg1", 256, DFF), ("wu1", 256, DFF), ("wd1", 704, D)]
    for l in range(depth):
        for nm, rows, cols in wspec:
            wshard[(l, nm)] = dram(f"{nm}_{l}", [rows, cols], F32, "ExternalInput")
            wfull[(l, nm)] = (dram(f"sh_{nm}_{l}", [rows, cols], BF16), dram(f"W_{nm}_{l}", [rows * 8, cols], BF16))
    out_ext = dram("out", [TOK, D], F32, "ExternalOutput")

    xres = dram("xres", [TOK, D], F32)
    qA = dram("qA", [512, TOK], BF16)
    qB = dram("qB", [512, TOK], BF16)
    qC = dram("qC", [512, TOK], BF16)
    qD = dram("qD", [512, TOK], BF16)
    iqT = dram("iqT", [1024, TOK], BF16)
    iwD = dram("iwD", [TOK, 16], F32)
    gC = dram("gC", [TOK, 512], F32)
    sndT = [dram(f"sndT{p}", [T_ROWS, 256], BF16) for p in range(8)]
    sndV = [dram(f"sndV{p}", [256, V_COLS], BF16) for p in range(8)]
    sndF = dram("sndF", [8, TOK], F32)
    rcvT = [dram(f"rcvT{p}", [4 * T_ROWS, 256], BF16) for p in range(8)]
    rcvV = [dram(f"rcvV{p}", [4 * 256, V_COLS], BF16) for p in range(8)]
    rcvF = dram("rcvF", [32, TOK], F32)
    cumS = dram("cumS", [8, 3, SEQ], BF16)
    ybuf = dram("ybuf", [TOK, D], BF16)

    B = Buf
    b_xres = [B(f"xres{t}") for t in range(4)]
    b_q = {k: B(k) for k in ("qA", "qB", "qC", "qD", "iqT", "iwD", "gC")}
    b_snd = B("snd")
    b_rcv = B("rcv")
    b_cumS = B("cumS")
    b_ybuf = B("ybuf")
    b_w = {k: B(f"w{k}") for k in wfull}

    ARENA_W = 52000
    arena_t = nc.alloc_sbuf_tensor("arena", [128, ARENA_W], F32)
    AR = Arena(arena_t[:, :], ARENA_W)
    ps_t = nc.alloc_psum_tensor("ps", [128, 8, 512], F32)
    PS = [ps_t[:, b, :] for b in range(8)]
    PSB = [ps_t[:, b, :].bitcast(BF16) for b in range(8)]
    b_ps = [B(f"ps{b}") for b in range(8)]

    def tile(shape, dt, name):
        return AR.alloc(shape, dt), B(name)

    cst, b_cst = tile([1024], F32, "cst")
    cstb, b_cstb = tile([1024], BF16, "cstb")
    tabbc, b_tab = tile([512], F32, "tabbc")
    biasA, b_biasA = tile([2, 8, 128], BF16, "biasA")
    biasB, b_biasB = tile([2, 8, 128], BF16, "biasB")
    condT, b_cond = tile([KC], BF16, "condT")
    modTs = [tile([144], F32, f"modT{i}") for i in range(2)]
    modAs = [tile([3, KC], F32, f"modA{i}") for i in range(2)]
    ngT, b_ng = tile([3, KC], F32, "ngT")
    qkgT, b_qkg = tile([6], F32, "qkgT")
    fbT, b_fb = tile([1], F32, "fbT")
    esink, b_esink = tile([8], F32, "esink")
    small, b_small = tile([64], F32, "small")
    ID = cst[:, K_ID:K_ID + 128]
    ONES = cst[:, K_ONES:K_ONES + 128]
    IDb = cstb[:, K_ID:K_ID + 128]
    TINCb = cstb[:, K_TINC:K_TINC + 128]
    NONESb_off = K_CAUSN
    CAUSTb = cstb[:, K_CAUST:K_CAUST + 128]
    STRNEGb = cstb[:, K_STRNEG:K_STRNEG + 128]
    BLKb = cstb[:, K_BLK:K_BLK + 128]
    NONESb = cstb[:, NONESb_off:NONESb_off + 128]
    CAUSN = cst[:, K_CAUSN:K_CAUSN + 128]
    STRICT = cst[:, K_STRICT:K_STRICT + 128]
    PERS = AR.mark()

    def V(eng, fn, reads=(), writes=()):
        return P.op(eng, fn, reads, writes)

    def setup():
        m = AR.mark()
        big, b_big = tile([NCONST], F32, "constsbig")
        P.dma("sync", lambda e: e.dma_start(out=big, in_=consts_in), writes=[b_big])
        V("vector", lambda e: e.tensor_copy(out=cst, in_=big[:, 0:1024]), [b_big], [b_cst])
        V("vector", lambda e: e.tensor_copy(out=cstb, in_=big[:, 0:1024]), [b_big], [b_cstb])
        V("vector", lambda e: e.memset(NONESb, -1.0), [b_cstb], [b_cstb])
        P.dma("sync", lambda e: e.dma_start(out=tabbc, in_=tab_in.rearrange("b h -> (b h)").partition_broadcast(128)),
              writes=[b_tab])
        tabd, b_tabd = tile([512], F32, "tabd")
        V("vector", lambda e: e.tensor_copy(out=tabd, in_=tabbc), [b_tab], [b_tabd])
        for b in range(32):
            V("vector", lambda e, b=b: e.tensor_tensor(out=tabd[:, b * 16:b * 16 + 8], in0=tabbc[:, b * 16:b * 16 + 8],
                                                     in1=tabbc[:, 496:504], op=ALU.subtract), [b_tab, b_tabd], [b_tabd])
        acc, b_acc = tile([2, 128], F32, "biasacc")
        for mix, (bt, bb) in enumerate(((biasA, b_biasA), (biasB, b_biasB))):
            eng = "vector"
            for typ in range(2):
                for h in range(8):
                    a = acc[:, mix, :]
                    if mix == 0:
                        V(eng, lambda e, a=a: e.memset(a, 0.0), [], [b_acc])
                    else:
                        V(eng, lambda e, a=a, typ=typ: e.tensor_scalar(a, big[:, K_INV + typ * 128:K_INV + typ * 128 + 128],
                                                                       NEG, None, ALU.mult), [b_big], [b_acc])
                    for b in range(32):
                        if typ == 1 and b == 0:
                            continue
                        oh = big[:, K_OH + (typ * 32 + b) * 128:K_OH + (typ * 32 + b) * 128 + 128]
                        sc = tabd[:, b * 16 + mix * 8 + h:b * 16 + mix * 8 + h + 1]
                        V(eng, lambda e, a=a, oh=oh, sc=sc: e.scalar_tensor_tensor(out=a, in0=oh, scalar=sc, in1=a,
                                                                                   op0=ALU.mult, op1=ALU.add),
                          [b_big, b_tabd, b_acc], [b_acc])
                    V(eng, lambda e, a=a, bt=bt, typ=typ, h=h: e.tensor_copy(out=bt[:, typ, h, :], in_=a), [b_acc], [bb])
        ctmp, b_ctmp = tile([KC], F32, "ctmp")
        P.dma("sync", lambda e: e.dma_start(out=ctmp, in_=cT_in), writes=[b_ctmp])
        V("scalar", lambda e: e.activation(out=condT, in_=ctmp, func=AF.Silu), [b_ctmp], [b_cond])
        P.barrier()
        AR.release(m)

    def prep_weights(l):
        for nm, rows, cols in wspec:
            src = wshard[(l, nm)]
            sh, full = wfull[(l, nm)]
            bsh = B(f"sh{l}{nm}")
            step = rows // 8
            for k in range(8):
                P.dma("gpsimd", lambda e, k=k, src=src, sh=sh, step=step: e.dma_start(
                    out=sh[k * step:(k + 1) * step, :], in_=src[k * step:(k + 1) * step, :]), writes=[bsh])
            P.cc(lambda e, sh=sh, full=full: e.collective_compute("AllGather", ALU.bypass, replica_groups=[list(range(8))],
                                                                  ins=[sh.opt()], outs=[full.opt()]),
                 reads=[bsh], writes=[b_w[(l, nm)]])

    def W(l, nm):
        return wfull[(l, nm)][1], b_w[(l, nm)]

    def compute_mod(l):
        m = AR.mark()
        wb = [tile([KC, 512], BF16, f"modw{i}") for i in range(2)]
        btmp, b_btmp = tile([144], F32, "badatmp")
        P.dma("sync", lambda e: e.dma_start(out=btmp, in_=bada_in[l]), writes=[b_btmp])
        P.dma("sync", lambda e: e.dma_start(out=ngT, in_=ng_in[l].rearrange("a p k -> p a k")), writes=[b_ng])
        gi = 0
        for half in range(2):
            wd, bw = W(l, f"wada{half}")
            for g in range(18):
                wt, bwt = wb[gi % 2]
                gi += 1
                P.dma("sync", lambda e, wt=wt, wd=wd, g=g: e.dma_start(
                    out=wt, in_=wd[:, g * 512:(g + 1) * 512].rearrange("(kc p) n -> p kc n", p=128)), [bw], [bwt])
                for jj in range(4):
                    j = half * 72 + g * 4 + jj
                    for kc in range(KC):
                        V("tensor", lambda e, wt=wt, jj=jj, kc=kc, j=j: e.matmul(
                            PS[7][:, j:j + 1], lhsT=wt[:, kc, jj * 128:(jj + 1) * 128], rhs=condT[:, kc:kc + 1],
                            start=(kc == 0), stop=(kc == KC - 1)), [bwt, b_cond], [b_ps[7]])
        modT, b_mod = modTs[l % 2]
        modA, b_modA = modAs[l % 2]
        V("vector", lambda e: e.tensor_tensor(out=modT, in0=PS[7][:, 0:144], in1=btmp, op=ALU.add),
          [b_ps[7], b_btmp], [b_mod])
        for i in range(3):
            V("vector", lambda e, i=i: e.scalar_tensor_tensor(
                out=modA[:, i, :], in0=modT[:, (3 * i + 1) * 16:(3 * i + 2) * 16], scalar=1.0, in1=ngT[:, i, :],
                op0=ALU.add, op1=ALU.mult), [b_mod, b_ng], [b_modA])
        P.dma("sync", lambda e: e.dma_start(out=qkgT, in_=qkg_in[l]), writes=[b_qkg])
        P.dma("sync", lambda e: e.dma_start(out=fbT[0:8], in_=fb_in[l]), writes=[b_fb])
        V("vector", lambda e: e.tensor_scalar(fbT[0:8], fbT[0:8], -1.0, None, ALU.mult), [b_fb], [b_fb])
        for i in (0, 2, 4):
            V("vector", lambda e, i=i: e.tensor_scalar(qkgT[:, i:i + 1], qkgT[:, i:i + 1], 0.125, None, ALU.mult),
              [b_qkg], [b_qkg])
        P.barrier()
        AR.release(m)

    def mod_shift(l, i):
        return modTs[l % 2][0][:, (3 * i) * 16:(3 * i + 1) * 16]

    def mod_gate(l, i):
        return modTs[l % 2][0][:, (3 * i + 2) * 16:(3 * i + 3) * 16]

    class Row:
        pass

    def row_alloc():
        R = Row()
        R.hT = [tile([KC, 512], BF16, f"hT{i}") for i in range(2)]
        R.hid = tile([FC, 512], BF16, "hid")
        R.wp = [tile([KC, 512], BF16, f"wp{i}") for i in range(3)]
        R.wi = 0
        R.xs = tile([4, D], F32, "xs")
        R.xn = tile([D], BF16, "xn")
        R.gbc = tile([D], F32, "gbc")
        R.st16 = [tile([512], BF16, f"st16_{i}") for i in range(3)]
        R.st32 = [tile([512], F32, f"st32_{i}") for i in range(3)]
        R.s16 = 0
        R.s32 = 0
        R.dg = tile([128], F32, "dg")
        R.psi = 0
        return R

    def nxt_w(R):
        t = R.wp[R.wi % 3]
        R.wi += 1
        return t

    def nxt16(R):
        t = R.st16[R.s16 % 3]
        R.s16 += 1
        return t

    def nxt32(R):
        t = R.st32[R.s32 % 3]
        R.s32 += 1
        return t

    def build_gate_bc(R, l, i, coef):
        gbc, b_gbc = R.gbc
        dg, b_dg = R.dg
        for kc in range(KC):
            V("vector", lambda e, kc=kc: e.tensor_scalar(dg, ID, mod_gate(l, i)[:, kc:kc + 1], coef, ALU.mult, ALU.mult),
              [b_cst, modTs[l % 2][1]], [b_dg])
            bank = 6 + (kc // 4) % 2
            V("tensor", lambda e, kc=kc, bank=bank: e.matmul(PS[bank][:, (kc % 4) * 128:(kc % 4 + 1) * 128], lhsT=ONES, rhs=dg,
                                                             start=True, stop=True), [b_dg, b_cst], [b_ps[bank]])
            if kc % 4 == 3:
                V("vector", lambda e, kc=kc, bank=bank: e.tensor_copy(out=gbc[:, (kc - 3) * 128:(kc + 1) * 128], in_=PS[bank]),
                  [b_ps[bank]], [b_gbc])

    def norm_to_hT(R, xrow, b_x, hT_out, tb, modl, modi):
        hT, b_hT = hT_out
        xn, b_xn = R.xn
        junk, b_junk = nxt32(R)
        ss = small[:, 0:1]
        rs = small[:, 1:2]
        V("scalar", lambda e: e.activation(out=xn, in_=xrow, func=AF.Square, accum_out=ss), [b_x], [b_xn, b_small])
        V("scalar", lambda e: e.activation(out=rs, in_=ss, func=AF.Sqrt, bias=EPS, scale=1.0 / D), [b_small], [b_small])
        V("vector", lambda e: e.reciprocal(rs, rs), [b_small], [b_small])
        V("scalar", lambda e: e.activation(out=xn, in_=xrow, func=AF.Copy, scale=rs), [b_x, b_small, b_xn], [b_xn])
        for half in range(2):
            bank = 4 + half
            for k8 in range(8):
                kc = half * 8 + k8
                V("tensor", lambda e, kc=kc, k8=k8, bank=bank: e.transpose(PSB[bank][:, k8 * 128:(k8 + 1) * 128],
                                                                         xn[:, kc * 128:(kc + 1) * 128], IDb),
                  [b_xn, b_cstb], [b_ps[bank]])
            for k8 in range(8):
                kc = half * 8 + k8
                V("scalar", lambda e, kc=kc, k8=k8, bank=bank: e.activation(
                    out=hT[:, kc, tb * 128:(tb + 1) * 128], in_=PSB[bank][:, k8 * 128:(k8 + 1) * 128], func=AF.Identity,
                    bias=mod_shift(modl, modi)[:, kc:kc + 1], scale=modAs[modl % 2][0][:, modi, kc:kc + 1]),
                  [b_ps[bank], modTs[modl % 2][1], modAs[modl % 2][1]], [b_hT])

    def load_x_tile(R, t, src):
        xs, b_xs = R.xs
        P.dma("sync", lambda e: e.dma_start(out=xs, in_=src[t * 512:(t + 1) * 512, :].rearrange("(b p) d -> p b d", p=128)),
              [b_xres[t]], [b_xs])

    def proj_residual(R, t, lhs_fn, b_lhs, nK, wd, bw, gate_l, gate_i, coef, hT_out, modl_next, modi_next, dst):
        xs, b_xs = R.xs
        gbc, b_gbc = R.gbc
        build_gate_bc(R, gate_l, gate_i, coef)
        load_x_tile(R, t, xres)
        npiece = 4 if nK == FC else 2
        pk = nK // npiece
        for q in range(4):
            for pc in range(npiece):
                wt, bwt = nxt_w(R)
                P.dma("sync", lambda e, wt=wt, pc=pc, q=q: e.dma_start(
                    out=wt[:, 0:pk, :], in_=wd[pc * pk * 128:(pc + 1) * pk * 128, q * 512:(q + 1) * 512].rearrange(
                        "(kc p) n -> p kc n", p=128)), [bw], [bwt])
                for tb in range(4):
                    for k in range(pk):
                        kk = pc * pk + k
                        V("tensor", lambda e, tb=tb, k=k, kk=kk, wt=wt: e.matmul(
                            PS[tb], lhsT=lhs_fn(kk, tb), rhs=wt[:, k, :], start=(kk == 0), stop=(kk == nK - 1)),
                          [bwt, b_lhs], [b_ps[tb]])
            for tb in range(4):
                tmp, b_tmp = nxt32(R)
                V("vector", lambda e, tb=tb, q=q, tmp=tmp: e.tensor_tensor(out=tmp, in0=PS[tb], in1=gbc[:, q * 512:(q + 1) * 512],
                                                                         op=ALU.mult), [b_ps[tb], b_gbc], [b_tmp])
                V("gpsimd", lambda e, tb=tb, q=q, tmp=tmp: e.tensor_tensor(out=xs[:, tb, q * 512:(q + 1) * 512], in0=tmp,
                                                                         in1=xs[:, tb, q * 512:(q + 1) * 512], op=ALU.add),
                  [b_tmp, b_xs], [b_xs])
        P.dma("sync", lambda e: e.dma_start(out=dst[t * 512:(t + 1) * 512, :].rearrange("(b p) d -> p b d", p=128), in_=xs),
              [b_xs], [b_xres[t]])
        if hT_out is not None:
            for tb in range(4):
                norm_to_hT(R, xs[:, tb, :], b_xs, hT_out, tb, modl_next, modi_next)

    def ffn_hidden(R, l, j, hT_in):
        hT, b_hT = hT_in
        hid, b_hid = R.hid
        wg, bwg = W(l, f"wg{j}")
        wu, bwu = W(l, f"wu{j}")
        for g in range(11):
            wgt, bwgt = nxt_w(R)
            P.dma("sync", lambda e, wgt=wgt, g=g: e.dma_start(
                out=wgt, in_=wg[:, g * 512:(g + 1) * 512].rearrange("(kc p) n -> p kc n", p=128)), [bwg], [bwgt])
            wut, bwut = nxt_w(R)
            P.dma("sync", lambda e, wut=wut, g=g: e.dma_start(
                out=wut, in_=wu[:, g * 512:(g + 1) * 512].rearrange("(kc p) n -> p kc n", p=128)), [bwu], [bwut])
            for c4 in range(4):
                fc = g * 4 + c4
                bg, bu = (0, 1) if fc % 2 == 0 else (2, 3)
                for kc in range(KC):
                    V("tensor", lambda e, kc=kc, c4=c4, wgt=wgt, bg=bg: e.matmul(
                        PS[bg], lhsT=wgt[:, kc, c4 * 128:(c4 + 1) * 128], rhs=hT[:, kc, :], start=(kc == 0), stop=(kc == KC - 1)),
                      [bwgt, b_hT], [b_ps[bg]])
                for kc in range(KC):
                    V("tensor", lambda e, kc=kc, c4=c4, wut=wut, bu=bu: e.matmul(
                        PS[bu], lhsT=wut[:, kc, c4 * 128:(c4 + 1) * 128], rhs=hT[:, kc, :], start=(kc == 0), stop=(kc == KC - 1)),
                      [bwut, b_hT], [b_ps[bu]])
                sg, b_sg = nxt32(R)
                V("scalar", lambda e, sg=sg, bg=bg: e.activation(out=sg, in_=PS[bg], func=AF.Silu), [b_ps[bg]], [b_sg])
                V("vector", lambda e, sg=sg, bu=bu, fc=fc: e.tensor_tensor(out=hid[:, fc, :], in0=PS[bu], in1=sg, op=ALU.mult),
                  [b_ps[bu], b_sg], [b_hid])

    FM = "fm"
    TMJ = "tm"
    IN_GROUPS = [
        (C_AQ, 512, [(FM, 0, 128, 0, qA, "qA", 0), (FM, 128, 128, 0, qA, "qA", 128), (FM, 256, 128, 0, qA, "qA", 256),
                     (FM, 384, 128, 0, qA, "qA", 384)]),
        (C_AK, 128, [(FM, 0, 64, 1, sndT, "snd", T_AK), (TMJ, 64, 64, "copy", sndV, "snd", V_A)]),
        (C_AIQ, 512, [(FM, i * 128, 128, None, iqT, "iqT", i * 128) for i in range(4)]),
        (C_AIQ + 512, 512, [(FM, i * 128, 128, None, iqT, "iqT", 512 + i * 128) for i in range(4)]),
        (C_AIK, 80, [(FM, 0, 64, None, sndT, "snd", T_AIK), (TMJ, 64, 16, "iw", iwD, "iwD", 0)]),
        (C_BQ, 512, [(FM, i * 128, 128, 2, qB, "qB", i * 128) for i in range(4)]),
        (C_BK, 128, [(FM, 0, 64, 3, sndT, "snd", T_BK), (TMJ, 64, 64, "copy", sndV, "snd", V_B)]),
        (C_CQ, 512, [(FM, i * 128, 128, 4, qC, "qC", i * 128) for i in range(4)]),
        (C_CK, 512, [(FM, i * 128, 128, 5, sndT, "snd", T_CK + i * 128) for i in range(4)]),
        (C_CV, 512, [(TMJ, 0, 512, "copy", sndV, "snd", V_C)]),
        (C_CF, 8, [(FM, 0, 8, "logf", sndF, "snd", 0)]),
        (C_CG, 512, [(TMJ, 0, 512, "sig", gC, "gC", 0)]),
        (C_DQ, 512, [(FM, i * 128, 128, "scale", qD, "qD", i * 128) for i in range(4)]),
        (C_DK, 512, [(FM, i * 128, 128, None, sndT, "snd", T_DK + i * 128) for i in range(4)]),
        (C_DV, 512, [(TMJ, 0, 512, "copy", sndV, "snd", V_D)]),
    ]

    def in_proj(R, l, t, hT_in):
        hT, b_hT = hT_in
        wd, bw = W(l, "win")
        t0 = t * 512
        for c0, ncol, units in IN_GROUPS:
            wt, bwt = nxt_w(R)
            P.dma("sync", lambda e, wt=wt, c0=c0, ncol=ncol: e.dma_start(
                out=wt[:, :, 0:ncol], in_=wd[:, c0:c0 + ncol].rearrange("(kc p) n -> p kc n", p=128)), [bw], [bwt])
            for kind, o, n, mode, dst, dkey, doff in units:
                bdst = b_snd if dkey == "snd" else b_q[dkey]
                if kind == FM:
                    bank = R.psi % 4
                    R.psi += 1
                    pb = PS[bank][0:n, :]
                    for kc in range(KC):
                        V("tensor", lambda e, kc=kc, o=o, n=n, wt=wt, pb=pb: e.matmul(
                            pb, lhsT=wt[:, kc, o:o + n], rhs=hT[:, kc, :], start=(kc == 0), stop=(kc == KC - 1)),
                          [bwt, b_hT], [b_ps[bank]])
                    if mode == "logf":
                        e1, b_e1 = nxt32(R)
                        V("scalar", lambda e, e1=e1, pb=pb: e.activation(out=e1[0:8], in_=pb, func=AF.Exp, bias=fbT[0:8], scale=-1.0),
                          [b_ps[bank], b_fb], [b_e1])
                        V("scalar", lambda e, e1=e1: e.activation(out=e1[0:8], in_=e1[0:8], func=AF.Ln, bias=1.0, scale=1.0),
                          [b_e1], [b_e1])
                        V("vector", lambda e, e1=e1: e.tensor_scalar(e1[0:8], e1[0:8], -1.0, None, ALU.mult), [b_e1], [b_e1])
                        P.dma("sync", lambda e, e1=e1: e.dma_start(out=sndF[:, t0:t0 + 512], in_=e1[0:8]), [b_e1], [bdst])
                        continue
                    st, b_st = nxt16(R)
                    if mode is None:
                        V("scalar", lambda e, st=st, pb=pb, n=n: e.copy(out=st[0:n], in_=pb), [b_ps[bank]], [b_st])
                    elif mode == "scale":
                        V("scalar", lambda e, st=st, pb=pb, n=n: e.mul(out=st[0:n], in_=pb, mul=0.125), [b_ps[bank]], [b_st])
                    else:
                        sq, b_sq = nxt16(R)
                        V("scalar", lambda e, sq=sq, pb=pb, n=n: e.activation(out=sq[0:n], in_=pb, func=AF.Square),
                          [b_ps[bank]], [b_sq])
                        sb = 6 + (R.psi % 2)
                        V("tensor", lambda e, sq=sq, n=n, sb=sb: e.matmul(PS[sb][0:n, :], lhsT=BLKb[0:n, 0:n], rhs=sq[0:n],
                                                                         start=True, stop=True), [b_sq, b_cstb], [b_ps[sb]])
                        rs, b_rs = nxt32(R)
                        V("scalar", lambda e, rs=rs, n=n, sb=sb: e.activation(out=rs[0:n], in_=PS[sb][0:n, :], func=AF.Sqrt,
                                                                             bias=EPS, scale=1.0), [b_ps[sb]], [b_rs])
                        V("vector", lambda e, rs=rs, n=n: e.reciprocal(rs[0:n], rs[0:n]), [b_rs], [b_rs])
                        V("vector", lambda e, st=st, pb=pb, rs=rs, n=n, mode=mode: e.scalar_tensor_tensor(
                            out=st[0:n], in0=pb, scalar=qkgT[0:n, mode:mode + 1], in1=rs[0:n], op0=ALU.mult, op1=ALU.mult),
                          [b_ps[bank], b_rs, b_qkg], [b_st])
                    if dst is sndT:
                        for hp in range(2):
                            P.dma("sync", lambda e, st=st, n=n, doff=doff, hp=hp: e.dma_start(
                                out=sndT[2 * t + hp][doff:doff + n, :], in_=st[0:n, hp * 256:(hp + 1) * 256]), [b_st], [bdst])
                    else:
                        P.dma("sync", lambda e, st=st, n=n, dst=dst, doff=doff: e.dma_start(
                            out=dst[doff:doff + n, t0:t0 + 512], in_=st[0:n]), [b_st], [bdst])
                else:
                    for tb in range(4):
                        bank = 4 + (R.psi % 2)
                        R.psi += 1
                        pb = PS[bank][:, 0:n]
                        for kc in range(KC):
                            V("tensor", lambda e, kc=kc, o=o, n=n, wt=wt, pb=pb, tb=tb: e.matmul(
                                pb, lhsT=hT[:, kc, tb * 128:(tb + 1) * 128], rhs=wt[:, kc, o:o + n], start=(kc == 0),
                                stop=(kc == KC - 1)), [bwt, b_hT], [b_ps[bank]])
                        r0 = t0 + tb * 128
                        if mode == "copy":
                            st, b_st = nxt16(R)
                            V("vector", lambda e, st=st, pb=pb, n=n: e.tensor_copy(out=st[:, 0:n], in_=pb), [b_ps[bank]], [b_st])
                        elif mode == "iw":
                            st, b_st = nxt32(R)
                            V("vector", lambda e, st=st, pb=pb, n=n: e.tensor_scalar(st[:, 0:n], pb, 0.03125, None, ALU.mult),
                              [b_ps[bank]], [b_st])
                        else:
                            st, b_st = nxt32(R)
                            V("scalar", lambda e, st=st, pb=pb, n=n: e.activation(out=st[:, 0:n], in_=pb, func=AF.Sigmoid),
                              [b_ps[bank]], [b_st])
                        if dst is sndV:
                            bi = t * 4 + tb
                            P.dma("sync", lambda e, st=st, n=n, doff=doff, bi=bi: e.dma_start(
                                out=sndV[bi // 2][(bi % 2) * 128:(bi % 2) * 128 + 128, doff:doff + n], in_=st[:, 0:n]), [b_st], [bdst])
                        else:
                            P.dma("sync", lambda e, st=st, n=n, dst=dst, doff=doff, r0=r0: e.dma_start(
                                out=dst[r0:r0 + 128, doff:doff + n], in_=st[:, 0:n]), [b_st], [bdst])

    def load_yT(R, t, hT_out):
        hT, b_hT = hT_out
        xn, b_xn = R.xn
        for tb in range(4):
            r0 = t * 512 + tb * 128
            P.dma("sync", lambda e, r0=r0: e.dma_start(out=xn, in_=ybuf[r0:r0 + 128, :]), [b_ybuf], [b_xn])
            for half in range(2):
                bank = 4 + half
                for k8 in range(8):
                    kc = half * 8 + k8
                    V("tensor", lambda e, kc=kc, k8=k8, bank=bank: e.transpose(PSB[bank][:, k8 * 128:(k8 + 1) * 128],
                                                                             xn[:, kc * 128:(kc + 1) * 128], IDb),
                      [b_xn, b_cstb], [b_ps[bank]])
                V("vector", lambda e, half=half, bank=bank, tb=tb: e.tensor_copy(
                    out=hT[:, half * 8:(half + 1) * 8, tb * 128:(tb + 1) * 128],
                    in_=PSB[bank].rearrange("p (k t) -> p k t", t=128)), [b_ps[bank]], [b_hT])

    def row_phase(stage):
        m = AR.mark()
        R = row_alloc()
        finals = []
        for t in range(4):
            hA, hB = R.hT
            if stage == 0:
                load_x_tile(R, t, x_in)
                xs, b_xs = R.xs
                P.dma("sync", lambda e, t=t: e.dma_start(
                    out=xres[t * 512:(t + 1) * 512, :].rearrange("(b p) d -> p b d", p=128), in_=xs), [b_xs], [b_xres[t]])
                for tb in range(4):
                    norm_to_hT(R, xs[:, tb, :], b_xs, hA, tb, 0, 0)
            else:
                l = stage - 1
                load_yT(R, t, hA)
                wd, bw = W(l, "wout")
                proj_residual(R, t, lambda kk, tb: hA[0][:, kk, tb * 128:(tb + 1) * 128], hA[1], KC, wd, bw, l, 1, 1.0, hB, l, 2, xres)
                ffn_hidden(R, l, 1, hB)
                wd, bw = W(l, "wd1")
                last = stage == depth
                proj_residual(R, t, lambda kk, tb: R.hid[0][:, kk, tb * 128:(tb + 1) * 128], R.hid[1], FC, wd, bw, l, 2, 0.5,
                              None if last else hA, stage, 0, out_ext if last else xres)
                if last:
                    finals.append(P.ops["sync"][-1])
            if stage < depth:
                l = stage
                ffn_hidden(R, l, 0, hA)
                wd, bw = W(l, "wd0")
                proj_residual(R, t, lambda kk, tb: R.hid[0][:, kk, tb * 128:(tb + 1) * 128], R.hid[1], FC, wd, bw, l, 0, 0.5, hB, l, 1, xres)
                in_proj(R, l, t, hB)
        P.barrier()
        AR.release(m)
        return finals

    def exchange():
        grp = [[0, 1, 2, 3], [4, 5, 6, 7]]
        for s, r in list(zip(sndT, rcvT)) + list(zip(sndV, rcvV)) + [(sndF, rcvF)]:
            P.cc(lambda e, s=s, r=r: e.collective_compute("AllGather", ALU.bypass, replica_groups=grp,
                                                          ins=[s.opt()], outs=[r.opt()]), reads=[b_snd], writes=[b_rcv])

    def finalize_group(o_ap, b_o, grp, li, ggbc, b_gg, st, gate_tile=None):
        ss = small[:, 8:9]
        rs = small[:, 9:10]
        junk, b_junk, y16, b_y16 = st
        if gate_tile is not None:
            V("vector", lambda e: e.tensor_tensor(out=o_ap, in0=o_ap, in1=gate_tile[0], op=ALU.mult), [b_o, gate_tile[1]], [b_o])
        V("scalar", lambda e: e.activation(out=junk, in_=o_ap, func=AF.Square, accum_out=ss), [b_o], [b_junk, b_small])
        V("scalar", lambda e: e.activation(out=rs, in_=ss, func=AF.Sqrt, bias=EPS, scale=1.0 / 512), [b_small], [b_small])
        V("vector", lambda e: e.reciprocal(rs, rs), [b_small], [b_small])
        V("vector", lambda e: e.scalar_tensor_tensor(out=y16, in0=o_ap, scalar=rs, in1=ggbc[:, grp * 512:(grp + 1) * 512],
                                                     op0=ALU.mult, op1=ALU.mult), [b_o, b_small, b_gg], [b_y16])
        P.dma("sync", lambda e: e.dma_start(out=ybuf[li * 128:(li + 1) * 128, grp * 512:(grp + 1) * 512], in_=y16),
              [b_y16], [b_ybuf])


    flags_in = dram("flags", [128, 64], F32, "ExternalInput")
    flg, b_flg = AR.alloc([64], F32), B("flg")
    AR_PERS2 = AR.mark()

    def fl(par, cand, which):
        k = (par * 5 + cand) * 3 + which
        return flg[:, k:k + 1]

    def ohr(r):
        return flg[:, 32 + r:33 + r]

    def attention(l):
        m0 = AR.mark()
        P.dma("sync", lambda e: e.dma_start(out=flg, in_=flags_in), writes=[b_flg])
        ggbc, b_gg = tile([D], F32, "ggbc")
        P.dma("sync", lambda e: e.dma_start(out=ggbc, in_=gg_in[l].partition_broadcast(128)), writes=[b_gg])
        stg = tile([512], F32, "finstage") + tile([512], BF16, "finstage16")
        MCT, b_MCT = tile([2, 4, 128], BF16, "MCT")
        MST, b_MST = tile([2, 4, 128], BF16, "MST")
        MSM, b_MSM = tile([2, 4, 128], BF16, "MSM")
        MN, b_MN = tile([2, 4, 128], F32, "MN")
        for par in range(2):
            for pos in range(4):
                fd, fh = fl(par, pos + 1, 0), fl(par, pos + 1, 2)
                for (dst, bd, dm, hv, dt_) in ((MCT, b_MCT, cst[:, K_CAUST:K_CAUST + 128], NEG, 0),
                                               (MST, b_MST, cst[:, K_STRNEG:K_STRNEG + 128], NEG, 0),
                                               (MN, b_MN, CAUSN, -1e30, 0)):
                    tmp = stg[0][:, 0:128]
                    V("vector", lambda e, tmp=tmp, fh=fh, hv=hv: e.tensor_scalar(tmp, ONES, fh, hv, ALU.mult, ALU.mult),
                      [b_cst, b_flg], [stg[1]])
                    V("vector", lambda e, tmp=tmp, dm=dm, fd=fd, dst=dst, par=par, pos=pos: e.scalar_tensor_tensor(
                        out=dst[:, par, pos, :], in0=dm, scalar=fd, in1=tmp, op0=ALU.mult, op1=ALU.add),
                      [b_cst, b_flg, stg[1]], [bd])
                tmp = stg[0][:, 0:128]
                V("vector", lambda e, tmp=tmp, fd=fd: e.tensor_scalar(tmp, STRICT, fd, None, ALU.mult), [b_cst, b_flg], [stg[1]])
                V("vector", lambda e, fd=fd, fh=fh: e.tensor_tensor(out=small[:, 16:17], in0=fd, in1=fh, op=ALU.add), [b_flg], [b_small])
                V("vector", lambda e: e.tensor_scalar(small[:, 16:17], small[:, 16:17], -1.0, 1.0, ALU.mult, ALU.add),
                  [b_small], [b_small])
                V("vector", lambda e, tmp=tmp, par=par, pos=pos: e.tensor_scalar(MSM[:, par, pos, :], tmp, small[:, 16:17], None,
                                                                               ALU.add), [stg[1], b_small], [b_MSM])
        m1 = AR.mark()
        attn_ab(l, ggbc, b_gg, stg, MN, b_MN)
        P.barrier()
        AR.release(m1)
        attn_cd(l, ggbc, b_gg, stg, MCT, b_MCT, MST, b_MST, MSM, b_MSM)
        P.barrier()
        AR.release(m0)

    def load_chunkmajor_T(dst, bdst, row0, nrows):
        for r in range(4):
            for p in range(8):
                P.dma("sync", lambda e, r=r, p=p: e.dma_start(
                    out=dst[0:nrows, 2 * p:2 * p + 2, r, :], in_=rcvT[p][r * T_ROWS + row0:r * T_ROWS + row0 + nrows, :].rearrange(
                        "p (i t) -> p i t", t=128)), [b_rcv], [bdst])

    def load_chunkmajor_V(dst, bdst, col0):
        for r in range(4):
            for p in range(8):
                P.dma("sync", lambda e, r=r, p=p: e.dma_start(
                    out=dst[:, 2 * p:2 * p + 2, r, 0:64], in_=rcvV[p][r * 256:(r + 1) * 256, col0:col0 + 64].rearrange(
                        "(i p) c -> p i c", p=128)), [b_rcv], [bdst])

    def attn_ab(l, ggbc, b_gg, stg, MN, b_MN):
        KA, b_KA = tile([NLOC, 4, 128], BF16, "KA")
        IK, b_IK = tile([NLOC, 4, 128], BF16, "IK")
        VA, b_VA = tile([NLOC, 4, 65], BF16, "VA")
        KB, b_KB = tile([2, 4, 128], BF16, "KB")
        VB, b_VB = tile([2, 4, 65], BF16, "VB")
        V("vector", lambda e: e.memset(KA[64:65], 1.0), [], [b_KA])
        V("vector", lambda e: e.memset(VA[:, :, :, 64:65], 1.0), [], [b_VA])
        V("vector", lambda e: e.memset(VB[:, :, :, 64:65], 1.0), [], [b_VB])
        load_chunkmajor_T(KA, b_KA, T_AK, 64)
        load_chunkmajor_T(IK, b_IK, T_AIK, 64)
        load_chunkmajor_V(VA, b_VA, V_A)
        sk, b_sk = tile([8], F32, "sk")
        P.dma("sync", lambda e: e.dma_start(out=sk, in_=sinks_in[l].partition_broadcast(128)), writes=[b_sk])
        V("scalar", lambda e: e.activation(out=esink, in_=sk, func=AF.Exp), [b_sk], [b_esink])
        BA, b_BA = tile([2, 5, 1024], BF16, "BA")
        BBt, b_BBt = tile([2, 5, 1024], BF16, "BB")
        bA0 = biasA[:, 0].rearrange("p h q -> p (h q)")
        bA1 = biasA[:, 1].rearrange("p h q -> p (h q)")
        bB0 = biasB[:, 0].rearrange("p h q -> p (h q)")
        bB1 = biasB[:, 1].rearrange("p h q -> p (h q)")
        sc, b_sc = tile([NLOC * 512], F32, "sc")
        t1k, b_t1k = sc[:, 0:1024], b_sc
        for par in range(2):
            for cand in range(5):
                fd, fp = fl(par, cand, 0), fl(par, cand, 1)
                V("vector", lambda e, fd=fd: e.tensor_scalar(t1k, bA0, fd, None, ALU.mult), [b_biasA, b_flg], [b_t1k])
                V("vector", lambda e, fp=fp, par=par, cand=cand: e.scalar_tensor_tensor(
                    out=BA[:, par, cand, :], in0=bA1, scalar=fp, in1=t1k, op0=ALU.mult, op1=ALU.add),
                  [b_biasA, b_flg, b_t1k], [b_BA])
                V("vector", lambda e, fd=fd: e.tensor_scalar(t1k, bB0, fd, None, ALU.mult), [b_biasB, b_flg], [b_t1k])
                V("vector", lambda e, fp=fp: e.scalar_tensor_tensor(out=t1k, in0=bB1, scalar=fp, in1=t1k, op0=ALU.mult, op1=ALU.add),
                  [b_biasB, b_flg, b_t1k], [b_t1k])
                V("vector", lambda e, fd=fd, fp=fp: e.tensor_tensor(out=small[:, 17:18], in0=fd, in1=fp, op=ALU.add), [b_flg], [b_small])
                V("vector", lambda e: e.tensor_scalar(small[:, 17:18], small[:, 17:18], -NEG, NEG, ALU.mult, ALU.add),
                  [b_small], [b_small])
                V("vector", lambda e, par=par, cand=cand: e.tensor_scalar(BBt[:, par, cand, :], t1k, small[:, 17:18], None, ALU.add),
                  [b_t1k, b_small], [b_BBt])
        I4, b_I4 = tile([4, 128], BF16, "I4")
        for k in range(4):
            V("vector", lambda e, k=k: e.tensor_copy(out=I4[:, k, :], in_=ID), [b_cst], [b_I4])
        I4f = I4.rearrange("p a b -> p (a b)")
        NM, b_NM = tile([NLOC * 512], BF16, "NM")
        QAt, b_QA = tile([8, 128], BF16, "QAt")
        QBt, b_QB = tile([8, 128], BF16, "QBt")
        IQt, b_IQ = tile([16, 128], BF16, "IQt")
        iwt, b_iwt = tile([16], F32, "iwt")
        rl = [tile([512], F32, f"rl{i}") for i in range(2)]
        PT = [tile([512], BF16, f"PT{i}") for i in range(3)]
        oab, b_oab = tile([512], F32, "oab")
        bs, b_bs = tile([16], F32, "bs")
        for h in range(8):
            V("vector", lambda e, h=h: e.tensor_scalar(QAt[64:65, h, :], ONES[64:65, :], tabbc[64:65, 496 + h:497 + h], None, ALU.mult),
              [b_cst, b_tab], [b_QA])
        V("vector", lambda e: e.memset(KB[64:65], 1.0), [], [b_KB])
        pti = [0]
        rli = [0]

        def attn_block(i, par, Kt, b_K, Vt, b_V, Qt, b_Q, Kdim, cands, use_nm, Bias, b_Bias):
            first = True
            for (ch, pos, cand) in cands:
                for hh in range(2):
                    bank = hh
                    ops = []
                    V("tensor", lambda e, ch=ch, pos=pos, hh=hh, bank=bank: e.matmul(
                        PS[bank], lhsT=Kt[0:Kdim, ch, pos, :], rhs=Qt[0:Kdim, hh * 4:(hh + 1) * 4, :].rearrange("p h q -> p (h q)"),
                        start=True, stop=False), [b_K, b_Q], [b_ps[bank]])
                    if use_nm:
                        V("tensor", lambda e, ch=ch, pos=pos, bank=bank: e.matmul(
                            PS[bank], lhsT=NM[:, (ch * 4 + pos) * 128:(ch * 4 + pos + 1) * 128], rhs=I4f, start=False, stop=False),
                          [b_NM, b_I4], [b_ps[bank]])
                    if cand is not None:
                        V("tensor", lambda e, cand=cand, hh=hh, bank=bank: e.matmul(
                            PS[bank], lhsT=IDb, rhs=Bias[:, par, cand, hh * 512:(hh + 1) * 512], start=False, stop=True),
                          [b_Bias, b_cstb], [b_ps[bank]])
                    pt, b_pt = PT[pti[0] % 3]
                    pti[0] += 1
                    V("scalar", lambda e, pt=pt, bank=bank: e.activation(out=pt, in_=PS[bank], func=AF.Exp), [b_ps[bank]], [b_pt])
                    for h4 in range(4):
                        h = hh * 4 + h4
                        V("tensor", lambda e, pt=pt, h4=h4, ch=ch, pos=pos, hh=hh, first=first: e.matmul(
                            PS[2 + hh][:, h4 * 65:(h4 + 1) * 65], lhsT=pt[:, h4 * 128:(h4 + 1) * 128], rhs=Vt[:, ch, pos, :],
                            start=(first and h4 == 0), stop=False), [b_pt, b_V], [b_ps[2 + hh]])
                first = False

        def fin_ab(i, grp, add_sink):
            for hh in range(2):
                acc = PS[2 + hh][:, 0:260].rearrange("p (h c) -> p h c", c=65)
                den = small[:, 24:28]
                if add_sink:
                    V("vector", lambda e, acc=acc, hh=hh: e.tensor_tensor(out=den, in0=acc[:, :, 64], in1=esink[:, hh * 4:(hh + 1) * 4],
                                                                        op=ALU.add), [b_ps[2 + hh], b_esink], [b_small])
                else:
                    V("vector", lambda e, acc=acc: e.tensor_copy(out=den, in_=acc[:, :, 64]), [b_ps[2 + hh]], [b_small])
                V("vector", lambda e: e.reciprocal(den, den), [b_small], [b_small])
                for h4 in range(4):
                    V("vector", lambda e, acc=acc, h4=h4, hh=hh: e.tensor_scalar(
                        oab[:, (hh * 4 + h4) * 64:(hh * 4 + h4 + 1) * 64], acc[:, h4, 0:64], small[:, 24 + h4:25 + h4], None, ALU.mult),
                      [b_ps[2 + hh], b_small], [b_oab])
            finalize_group(oab, b_oab, grp, i, ggbc, b_gg, stg)

        for i in range(NLOC):
            par = i % 2
            t0 = i * 128
            nch = i + 1
            L = nch * 512
            P.dma("sync", lambda e, t0=t0: e.dma_start(out=IQt[0:64], in_=iqT.rearrange("(h d) t -> d h t", d=64)[:, :, t0:t0 + 128]),
                  [b_q["iqT"]], [b_IQ])
            P.dma("sync", lambda e, t0=t0: e.dma_start(out=iwt, in_=iwD[t0:t0 + 128, :]), [b_q["iwD"]], [b_iwt])
            P.dma("sync", lambda e, t0=t0: e.dma_start(out=QAt[0:64], in_=qA.rearrange("(h d) t -> d h t", d=64)[:, :, t0:t0 + 128]),
                  [b_q["qA"]], [b_QA])
            P.dma("sync", lambda e, t0=t0: e.dma_start(out=QBt[0:64], in_=qB.rearrange("(h d) t -> d h t", d=64)[:, :, t0:t0 + 128]),
                  [b_q["qB"]], [b_QB])
            for ch in range(nch):
                scc = sc[:, ch * 512:(ch + 1) * 512]
                for h in range(16):
                    bank = 4 + h % 4
                    V("tensor", lambda e, h=h, ch=ch, bank=bank: e.matmul(
                        PS[bank], lhsT=IQt[0:64, h, :], rhs=IK[0:64, ch].rearrange("p r t -> p (r t)"), start=True, stop=True),
                      [b_IQ, b_IK], [b_ps[bank]])
                    r_, b_r = rl[rli[0] % 2]
                    rli[0] += 1
                    V("scalar", lambda e, r_=r_, bank=bank: e.activation(out=r_, in_=PS[bank], func=AF.Relu), [b_ps[bank]], [b_r])
                    if h == 0:
                        V("vector", lambda e, r_=r_, scc=scc: e.tensor_scalar(scc, r_, iwt[:, 0:1], None, ALU.mult), [b_r, b_iwt], [b_sc])
                    else:
                        V("vector", lambda e, r_=r_, scc=scc, h=h: e.scalar_tensor_tensor(
                            out=scc, in0=r_, scalar=iwt[:, h:h + 1], in1=scc, op0=ALU.mult, op1=ALU.add), [b_r, b_iwt, b_sc], [b_sc])
            amax = bs[:, 0:1]
            V("vector", lambda e, L=L: e.tensor_reduce(out=amax, in_=sc[:, 0:L], axis=mybir.AxisListType.X, op=ALU.max,
                                                       apply_absolute_value=True), [b_sc], [b_bs])
            for pos in range(4):
                V("gpsimd", lambda e, pos=pos, i=i, par=par: e.tensor_tensor(
                    out=sc[:, (i * 4 + pos) * 128:(i * 4 + pos + 1) * 128], in0=sc[:, (i * 4 + pos) * 128:(i * 4 + pos + 1) * 128],
                    in1=MN[:, par, pos, :], op=ALU.add), [b_sc, b_MN], [b_sc])
            lo, w0, mid, cnt, tt, wk = (bs[:, k:k + 1] for k in range(1, 7))
            V("vector", lambda e: e.tensor_scalar(lo, amax, -1.0, -1.0, ALU.mult, ALU.add), [b_bs], [b_bs])
            V("vector", lambda e: e.tensor_scalar(w0, amax, 1.0, 0.5, ALU.mult, ALU.add), [b_bs], [b_bs])
            jk, b_jk = NM, b_NM
            for it in range(NBIS):
                ck = 0.5 ** it
                V("vector", lambda e, ck=ck: e.tensor_scalar(wk, w0, ck, None, ALU.mult), [b_bs], [b_bs])
                V("vector", lambda e: e.tensor_tensor(out=mid, in0=lo, in1=wk, op=ALU.add), [b_bs], [b_bs])
                V("vector", lambda e, L=L: e.tensor_scalar(jk[:, 0:L], sc[:, 0:L], mid, None, ALU.is_gt, ALU.add, accum_out=cnt),
                  [b_sc, b_bs], [b_jk, b_bs])
                V("vector", lambda e: e.tensor_scalar(tt, cnt, float(TOPK) - 0.5, wk, ALU.is_ge, ALU.mult), [b_bs], [b_bs])
                V("vector", lambda e: e.tensor_tensor(out=lo, in0=lo, in1=tt, op=ALU.add), [b_bs], [b_bs])
            V("vector", lambda e, L=L: e.tensor_scalar(NM[:, 0:L], sc[:, 0:L], lo, NEG, ALU.is_le, ALU.mult), [b_sc, b_bs], [b_NM])
            cands = []
            for ch in range(nch):
                for pos in range(4):
                    cand = None
                    if ch == i:
                        cand = pos + 1
                    elif ch == i - 1 and pos == (0 if par == 0 else 3):
                        cand = 0
                    cands.append((ch, pos, cand))
            attn_block(i, par, KA, b_KA, VA, b_VA, QAt, b_QA, 65, cands, True, BA, b_BA)
            fin_ab(i, 0, False)
            c_lo = max(i - 1, 0)
            s_lo = 1 - (i - c_lo)
            for r in range(4):
                for cch in range(c_lo, i + 1):
                    sl = cch - (i - 1)
                    P.dma("sync", lambda e, r=r, cch=cch, sl=sl: e.dma_start(
                        out=KB[0:64, sl, r, :], in_=rcvT[cch // 2][r * T_ROWS + T_BK:r * T_ROWS + T_BK + 64,
                                                                  (cch % 2) * 128:(cch % 2) * 128 + 128]), [b_rcv], [b_KB])
                    P.dma("sync", lambda e, r=r, cch=cch, sl=sl: e.dma_start(
                        out=VB[:, sl, r, 0:64], in_=rcvV[cch // 2][r * 256 + (cch % 2) * 128:r * 256 + (cch % 2) * 128 + 128,
                                                                   V_B:V_B + 64]), [b_rcv], [b_VB])
            cands = []
            if i >= 1:
                cands.append((0, 0 if par == 0 else 3, 0))
            for pos in range(4):
                cands.append((1, pos, pos + 1))
            attn_block(i, par, KB, b_KB, VB, b_VB, QBt, b_QB, 64, cands, False, BBt, b_BBt)
            fin_ab(i, 1, True)

    def fox_cumsum():
        m = AR.mark()
        lf, b_lf = tile([SEQ], F32, "lf", )
        cm, b_cm = tile([SEQ], F32, "cumCM")
        on, b_on = tile([SEQ], F32, "ones8k")
        lf8, cm8, on8 = lf[0:8], cm[0:8], on[0:8]
        lfv = lf8.rearrange("p (k m t) -> p k m t", m=8, t=128)
        for r in range(4):
            for par in range(2):
                mm = r if par == 0 else 7 - r
                P.dma("sync", lambda e, r=r, par=par, mm=mm: e.dma_start(
                    out=lfv[:, :, mm, :], in_=rcvF[r * 8:(r + 1) * 8, :].rearrange("p (k q t) -> p k q t", q=2, t=128)[:, :, par, :]),
                    [b_rcv], [b_lf])
        V("vector", lambda e: e.memset(on8, 1.0), [], [b_on])
        V("vector", lambda e: e.tensor_tensor_scan(out=on8, data0=on8, data1=lf8, initial=0.0, op0=ALU.mult, op1=ALU.add),
          [b_lf, b_on], [b_on])
        cum = on8.rearrange("p (k m t) -> p k m t", m=8, t=128)
        cmv = cm8.rearrange("p (i r t) -> p i r t", r=4, t=128).rearrange("p (k q) r t -> p k q r t", q=2)
        for r in range(4):
            for par in range(2):
                mm = r if par == 0 else 7 - r
                P.dma("sync", lambda e, r=r, par=par, mm=mm: e.dma_start(out=cmv[:, :, par, r, :], in_=cum[:, :, mm, :]), [b_on], [b_cm])
        cq, b_cq = tile([TOK], F32, "cumQ")
        cq8 = cq[0:8]
        cm4 = cm8.rearrange("p (i r t) -> p i r t", r=4, t=128)
        cqv = cq8.rearrange("p (i t) -> p i t", t=128)
        for r in range(4):
            if r == 0:
                V("vector", lambda e: e.tensor_scalar(cqv, cm4[:, :, 0, :], ohr(0)[0:8], None, ALU.mult), [b_cm, b_flg], [b_cq])
            else:
                V("vector", lambda e, r=r: e.scalar_tensor_tensor(out=cqv, in0=cm4[:, :, r, :], scalar=ohr(r)[0:8], in1=cqv,
                                                                  op0=ALU.mult, op1=ALU.add), [b_cm, b_flg, b_cq], [b_cq])
        sp16, b_sp16 = tile([SEQ], BF16, "split16")
        back, b_back = lf, b_lf
        for (src, bsrc, n, dstd, bdst) in ((cm8, b_cm, SEQ, cumS, b_cumS), (cq8, b_cq, TOK, cumQs, b_cumS)):
            for k in range(3):
                V("vector", lambda e, src=src, n=n: e.tensor_copy(out=sp16[0:8, 0:n], in_=src[:, 0:n]), [bsrc], [b_sp16])
                P.dma("sync", lambda e, k=k, n=n, dstd=dstd: e.dma_start(out=dstd[:, k, :], in_=sp16[0:8, 0:n]), [b_sp16], [bdst])
                if k < 2:
                    V("vector", lambda e, n=n: e.tensor_copy(out=back[0:8, 0:n], in_=sp16[0:8, 0:n]), [b_sp16], [b_back])
                    V("vector", lambda e, src=src, n=n: e.tensor_tensor(out=src[:, 0:n], in0=src[:, 0:n], in1=back[0:8, 0:n],
                                                                       op=ALU.subtract), [bsrc, b_back], [bsrc])
        P.barrier()
        AR.release(m)

    cumQs = dram("cumQs", [8, 3, TOK], BF16)

    def attn_cd(l, ggbc, b_gg, stg, MCT, b_MCT, MST, b_MST, MSM, b_MSM):
        fox_cumsum()
        KT = [tile([NLOC, 4, 128], BF16, f"KT{i}") for i in range(2)]
        VT = [tile([NLOC, 4, 65], BF16, f"VT{i}") for i in range(2)]
        QT = [tile([512], BF16, f"QT{i}") for i in range(2)]
        PT = [tile([512], BF16, f"PTc{i}") for i in range(3)]
        oall, b_oall = tile([NLOC, 512], F32, "oall")
        ex, b_ex = tile([512], F32, "sbexp")
        spt = [tile([512], BF16, f"sbsp{i}") for i in range(2)]
        us, b_us = tile([512], BF16, "usum")
        gt = tile([512], F32, "gateC")
        for kt, bk in KT:
            V("vector", lambda e, kt=kt: e.memset(kt[64:70], 1.0), [], [bk])
        for qt, bq in QT:
            V("vector", lambda e, qt=qt: e.memset(qt[64:70], -1.0), [], [bq])
        for vt, bv in VT:
            V("vector", lambda e, vt=vt: e.memset(vt[:, :, :, 64:65], 1.0), [], [bv])
        cnt = [0, 0, 0]
        for mixer in ("C", "D"):
            fox = mixer == "C"
            krow0 = T_CK if fox else T_DK
            vcol0 = V_C if fox else V_D
            qsrc, bqsrc = (qC, b_q["qC"]) if fox else (qD, b_q["qD"])
            Kdim = 70 if fox else 64
            ncol = 65 if fox else 64
            for h in range(8):
                kt, bk = KT[h % 2]
                vt, bv = VT[h % 2]
                load_chunkmajor_T(kt, bk, krow0 + h * 64, 64)
                load_chunkmajor_V(vt, bv, vcol0 + h * 64)
                if fox:
                    P.dma("sync", lambda e, kt=kt, h=h: e.dma_start(
                        out=kt[67:70].rearrange("p i r t -> p (i r t)"), in_=cumS[h]), [b_cumS], [bk])
                for g in range(4):
                    qt, bq = QT[cnt[0] % 2]
                    cnt[0] += 1
                    P.dma("sync", lambda e, qt=qt, h=h, g=g, qsrc=qsrc: e.dma_start(out=qt[0:64], in_=qsrc[h * 64:(h + 1) * 64, g * 512:(g + 1) * 512]),
                          [bqsrc], [bq])
                    if fox:
                        P.dma("sync", lambda e, qt=qt, h=h, g=g: e.dma_start(out=qt[64:67], in_=cumQs[h, :, g * 512:(g + 1) * 512]),
                              [b_cumS], [bq])
                    accb = 4 + (cnt[0] % 2)
                    nchunk = 4 * g + 4
                    order = [(ch, pos) for ch in range(nchunk) for pos in range(4)]
                    if not fox:
                        order = []
                        for ch in reversed(range(nchunk)):
                            ps_ = range(4) if ch % 2 == 1 else reversed(range(4))
                            order += [(ch, pos) for pos in ps_]
                        V("vector", lambda e: e.memset(us, 0.0), [], [b_us])
                    first = True
                    for (ch, pos) in order:
                        cc = ch - 4 * g
                        c0 = max(cc, 0)
                        cols = slice(c0 * 128, 512)
                        masked = cc >= 0
                        par = ch % 2
                        bank = cnt[1] % 4
                        cnt[1] += 1
                        V("tensor", lambda e, kt=kt, qt=qt, ch=ch, pos=pos, cols=cols, bank=bank, masked=masked, Kdim=Kdim: e.matmul(
                            PS[bank][:, cols], lhsT=kt[0:Kdim, ch, pos, :], rhs=qt[0:Kdim, cols], start=True, stop=False),
                          [bk, bq], [b_ps[bank]])
                        mcol = slice(c0 * 128, c0 * 128 + 128)
                        if fox:
                            if masked:
                                V("tensor", lambda e, bank=bank, mcol=mcol, par=par, pos=pos: e.matmul(
                                    PS[bank][:, mcol], lhsT=IDb, rhs=MCT[:, par, pos, :], start=False, stop=True),
                                  [b_MCT, b_cstb], [b_ps[bank]])
                        else:
                            sp_, b_sp = spt[cnt[2] % 2]
                            cnt[2] += 1
                            V("scalar", lambda e, bank=bank, cols=cols: e.activation(out=ex[:, cols], in_=PS[bank][:, cols], func=AF.Exp),
                              [b_ps[bank]], [b_ex])
                            V("scalar", lambda e, sp_=sp_, cols=cols: e.activation(out=sp_[:, cols], in_=ex[:, cols], func=AF.Ln, bias=1.0,
                                                                                   scale=1.0), [b_ex], [b_sp])
                            if masked:
                                V("vector", lambda e, sp_=sp_, mcol=mcol, par=par, pos=pos: e.tensor_tensor(
                                    out=sp_[:, mcol], in0=sp_[:, mcol], in1=MSM[:, par, pos, :], op=ALU.mult), [b_sp, b_MSM], [b_sp])
                            V("tensor", lambda e, bank=bank, cols=cols, sp_=sp_: e.matmul(
                                PS[bank][:, cols], lhsT=TINCb, rhs=sp_[:, cols], start=False, stop=False), [b_sp, b_cstb], [b_ps[bank]])
                            V("tensor", lambda e, bank=bank, cols=cols, masked=masked: e.matmul(
                                PS[bank][:, cols], lhsT=NONESb, rhs=us[:, cols], start=False, stop=not masked), [b_us, b_cstb], [b_ps[bank]])
                            if masked:
                                V("tensor", lambda e, bank=bank, mcol=mcol, par=par, pos=pos: e.matmul(
                                    PS[bank][:, mcol], lhsT=IDb, rhs=MST[:, par, pos, :], start=False, stop=True),
                                  [b_MST, b_cstb], [b_ps[bank]])
                            V("vector", lambda e, sp_=sp_, cols=cols: e.tensor_tensor(out=us[:, cols], in0=us[:, cols], in1=sp_[:, cols],
                                                                                     op=ALU.add), [b_us, b_sp], [b_us])
                        pt, b_pt = PT[cnt[1] % 3]
                        V("scalar", lambda e, pt=pt, bank=bank, cols=cols: e.activation(out=pt[:, cols], in_=PS[bank][:, cols], func=AF.Exp),
                          [b_ps[bank]], [b_pt])
                        for c in range(c0, 4):
                            V("tensor", lambda e, pt=pt, c=c, vt=vt, ch=ch, pos=pos, first=first, accb=accb, c0=c0, ncol=ncol: e.matmul(
                                PS[accb][:, c * 65:c * 65 + ncol], lhsT=pt[:, c * 128:(c + 1) * 128], rhs=vt[:, ch, pos, 0:ncol],
                                start=(first and c == c0), stop=False), [b_pt, bv], [b_ps[accb]])
                        first = False
                    acc = PS[accb][:, 0:260].rearrange("p (c k) -> p c k", k=65)
                    if fox:
                        den = small[:, 28:32]
                        V("vector", lambda e, acc=acc: e.reciprocal(den, acc[:, :, 64]), [b_ps[accb]], [b_small])
                        for c in range(4):
                            V("vector", lambda e, acc=acc, c=c, g=g, h=h: e.tensor_scalar(
                                oall[:, g * 4 + c, h * 64:(h + 1) * 64], acc[:, c, 0:64], small[:, 28 + c:29 + c], None, ALU.mult),
                              [b_ps[accb], b_small], [b_oall])
                    else:
                        V("vector", lambda e, acc=acc, g=g, h=h: e.tensor_copy(out=oall[:, g * 4:(g + 1) * 4, h * 64:(h + 1) * 64],
                                                                              in_=acc[:, :, 0:64]), [b_ps[accb]], [b_oall])
            for i in range(NLOC):
                if fox:
                    P.dma("sync", lambda e, i=i: e.dma_start(out=gt[0], in_=gC[i * 128:(i + 1) * 128, :]), [b_q["gC"]], [gt[1]])
                finalize_group(oall[:, i, :], b_oall, 2 if fox else 3, i, ggbc, b_gg, stg, gt if fox else None)

    def early(level):
        if stop > level:
            return False
        b_all = [b_xres[t] for t in range(4)]
        if level <= 4:
            fw = [P.dma("sync", lambda e: e.dma_start(out=out_ext, in_=xres if level >= 4 else x_in), b_all, [])]
        elif level == 5:
            fw = []
            fw.append(P.dma("gpsimd", lambda e: e.dma_start(out=out_ext[0:512, :], in_=qA), [b_q["qA"]], []))
            fw.append(P.dma("gpsimd", lambda e: e.dma_start(out=out_ext[512:1024, :], in_=qC), [b_q["qC"]], []))
            fw.append(P.dma("gpsimd", lambda e: e.dma_start(out=out_ext[1024:1536, :], in_=iqT[0:512, :]), [b_q["iqT"]], []))
            fw.append(P.dma("gpsimd", lambda e: e.dma_start(out=out_ext[1536:2048, 0:256], in_=rcvT[0][0:512, :]), [b_rcv], []))
            fw.append(P.dma("gpsimd", lambda e: e.dma_start(out=out_ext[1536:2048, 256:512], in_=rcvT[1][T_ROWS:T_ROWS + 512, :]), [b_rcv], []))
            fw.append(P.dma("gpsimd", lambda e: e.dma_start(out=out_ext[1536:1792, 512:512 + V_COLS], in_=rcvV[0][256:512, :]), [b_rcv], []))
        else:
            fw = [P.dma("gpsimd", lambda e: e.dma_start(out=out_ext, in_=ybuf), [b_ybuf], [])]
        P.emit(final_waits=fw)
        return True

    setup()
    if early(1):
        return nc
    prep_weights(0)
    if depth > 1:
        prep_weights(1)
    if early(2):
        return nc
    compute_mod(0)
    if early(3):
        return nc
    finals = []
    for stage in range(depth + 1):
        finals = row_phase(stage)
        if stage == 0 and early(4):
            return nc
        if stage < depth:
            exchange()
            if stage == 0 and early(5):
                return nc
            if stage + 2 < depth:
                prep_weights(stage + 2)
            if stage + 1 < depth:
                compute_mod(stage + 1)
            attention(stage)
            if stage == 0 and early(6):
                return nc
    P.emit(final_waits=finals)
    return nc


_PROG_CACHE = {}
_LAST_DBG = None


def _flags_for_rank(r):
    f = np.zeros((64,), np.float32)

    def setf(par, cand, which):
        f[(par * 5 + cand) * 3 + which] = 1.0
    for p in range(4):
        if p == r:
            setf(0, p + 1, 0)
        elif p == r - 1:
            setf(0, p + 1, 1)
        elif p > r:
            setf(0, p + 1, 2)
    if r == 0:
        setf(0, 0, 1)
    for p in range(4):
        if p == r:
            setf(1, p + 1, 0)
        elif p == r + 1:
            setf(1, p + 1, 1)
        elif p < r:
            setf(1, p + 1, 2)
    if r == 3:
        setf(1, 0, 1)
    f[32 + r] = 1.0
    return np.tile(f[None, :], (128, 1))


def _local_rows(r):
    rows = []
    for i in range(NLOC):
        j = loc2blk(r, i)
        rows.append(np.arange(j * 128, (j + 1) * 128))
    return np.concatenate(rows)


def kernel(x, c, w_ada, b_ada, norm_g, w_in, qk_g, forget_b, sinks, rel_table, group_g, w_out,
           w_ffn_gate, w_ffn_up, w_ffn_down, _depth=None, _stop=99):
    depth = int(_depth) if _depth is not None else int(w_ada.shape[0])
    f32 = lambda a: np.ascontiguousarray(np.asarray(a, dtype=np.float32))
    x, c, w_ada, b_ada, norm_g, w_in, qk_g, forget_b, sinks, rel_table, group_g, w_out = map(
        f32, (x, c, w_ada, b_ada, norm_g, w_in, qk_g, forget_b, sinks, rel_table, group_g, w_out))
    w_ffn_gate, w_ffn_up, w_ffn_down = map(f32, (w_ffn_gate, w_ffn_up, w_ffn_down))
    if (depth, _stop) not in _PROG_CACHE:
        _PROG_CACHE[(depth, _stop)] = build_program(depth, _stop)
    nc = _PROG_CACHE[(depth, _stop)]
    consts = make_consts()
    badaT = np.ascontiguousarray(b_ada[:depth].reshape(depth, 144, 128).transpose(0, 2, 1))
    normgT = np.ascontiguousarray(norm_g[:depth].reshape(depth, 3, KC, 128).transpose(0, 1, 3, 2))
    qkg = np.ascontiguousarray(np.tile(qk_g[:depth].transpose(0, 2, 1), (1, 2, 1)))
    fb = np.ascontiguousarray(forget_b[:depth].reshape(depth, 8, 1))
    in_maps = []
    for core in range(8):
        b, r = core // 4, core % 4
        rows = _local_rows(r)
        m = {
            "x": np.ascontiguousarray(x[b][rows]),
            "cT": np.ascontiguousarray(c[b].reshape(KC, 128).T),
            "badaT": badaT, "normgT": normgT, "qkg": qkg, "fb": fb,
            "sinks": np.ascontiguousarray(sinks[:depth]), "tab": rel_table,
            "gg": np.ascontiguousarray(group_g[:depth]), "consts": consts, "flags": _flags_for_rank(r),
        }
        for l in range(depth):
            def sh(a):
                n = a.shape[0] // 8
                return np.ascontiguousarray(a[core * n:(core + 1) * n])
            m[f"wada0_{l}"] = sh(w_ada[l][:, :9216])
            m[f"wada1_{l}"] = sh(w_ada[l][:, 9216:])
            m[f"win_{l}"] = sh(w_in[l])
            m[f"wout_{l}"] = sh(w_out[l])
            for j in range(2):
                m[f"wg{j}_{l}"] = sh(w_ffn_gate[l, j])
                m[f"wu{j}_{l}"] = sh(w_ffn_up[l, j])
                m[f"wd{j}_{l}"] = sh(w_ffn_down[l, j])
        in_maps.append(m)
    res = run_bass_kernel_spmd(nc, in_maps, core_ids=list(range(8)))
    out = np.zeros((2, SEQ, D), np.float32)
    global _LAST_DBG
    _LAST_DBG = [np.asarray(res.results[core]["out"]).astype(np.float32) for core in range(8)]
    for core in range(8):
        b, r = core // 4, core % 4
        out[b][_local_rows(r)] = np.asarray(res.results[core]["out"], dtype=np.float32)
    return out
```
